# Optimizing a Trainium2 kernel written in Bass

```python
import jax
import jax.numpy as jnp
from jax import lax
import numpy as np

D_MODEL = 1024
BATCH = 16
SEQ = 2048
DEPTH = 1

MEM_LEN = 256
GDN_HEADS = 4
GDN_DK = 128
GDN_DV = 128
GDN_CONV = 4
GDN_CHUNK = 64
SB_HEADS = 8
SB_DH = 64
SB_QBLOCK = 128
MEM_HEADS = 4
MEM_DH = 128
N_BRANCHES = 3
N_EXPERTS = 32
TOP_K = 4
D_FF = D_MODEL
SWIGLU_LIMIT = 7.0
SWIGLU_ALPHA = 1.702
MOE_BLOCK = 128
LN_EPS = 1e-5
NORM_EPS = 1e-6
DEEPNORM_ALPHA = (2 * DEPTH) ** 0.25
DEEPNORM_BETA = (8 * DEPTH) ** -0.25

GDN_QK = GDN_HEADS * GDN_DK
GDN_V = GDN_HEADS * GDN_DV
SB_W = SB_HEADS * SB_DH
MEM_W = MEM_HEADS * MEM_DH
GDN_CONV_CH = 2 * GDN_QK + GDN_V
IN_SIZES = (GDN_QK, GDN_QK, GDN_V, GDN_V, GDN_HEADS, GDN_HEADS, SB_W, SB_W, SB_W, MEM_W, N_BRANCHES * D_MODEL)
D_IN = sum(IN_SIZES)

kernel_name = 'hybrid_gdn_stickbreak_memxattn_moe_deepnorm'


def _layer_norm(x, g, b):
    xf = x.astype(jnp.float32)
    mu = jnp.mean(xf, -1, keepdims=True)
    var = jnp.mean(jnp.square(xf - mu), -1, keepdims=True)
    y = (xf - mu) * lax.rsqrt(var + LN_EPS) * g.astype(jnp.float32) + b.astype(jnp.float32)
    return y.astype(x.dtype)


def _l2norm(t):
    return t * lax.rsqrt(jnp.sum(t * t, -1, keepdims=True) + NORM_EPS)


def _heads(t, n):
    b, s, _ = t.shape
    return t.reshape(b, s, n, -1).transpose(0, 2, 1, 3)


def _causal_depthwise_conv(x, w):
    return lax.conv_general_dilated(
        x, w[:, None, :].astype(x.dtype), window_strides=(1,),
        padding=((w.shape[0] - 1, 0),), dimension_numbers=('NWC', 'WIO', 'NWC'),
        feature_group_count=x.shape[-1])


def _gated_delta_rule(q, k, v, g, beta):
    b, h, s, dk = q.shape
    dv = v.shape[-1]
    n = s // GDN_CHUNK
    q = q.reshape(b, h, n, GDN_CHUNK, dk)
    k = k.reshape(b, h, n, GDN_CHUNK, dk)
    v = v.reshape(b, h, n, GDN_CHUNK, dv)
    g = g.reshape(b, h, n, GDN_CHUNK)
    beta = beta.reshape(b, h, n, GDN_CHUNK)
    gc = jnp.cumsum(g, axis=-1)
    idx = jnp.arange(GDN_CHUNK)
    lower_incl = idx[:, None] >= idx[None, :]
    lower_strict = idx[:, None] > idx[None, :]
    diff = gc[..., :, None] - gc[..., None, :]
    decay = jnp.where(lower_incl, jnp.exp(jnp.where(lower_incl, diff, 0.0)), 0.0)
    kk = jnp.einsum('bhncd,bhnjd->bhncj', k, k)
    a_mat = jnp.where(lower_strict, kk * decay * beta[..., None], 0.0)
    rhs = jnp.concatenate([v * beta[..., None], k * (beta * jnp.exp(gc))[..., None]], axis=-1)
    sol = lax.linalg.triangular_solve(a_mat, rhs, left_side=True, lower=True, unit_diagonal=True)
    u, w = sol[..., :dv], sol[..., dv:]
    attn = jnp.einsum('bhncd,bhnjd->bhncj', q, k) * decay
    g_last = gc[..., -1]
    qg = q * jnp.exp(gc)[..., None]
    kd = k * jnp.exp(g_last[..., None] - gc)[..., None]

    def step(state, xs):
        qg_i, kd_i, u_i, w_i, attn_i, gl_i = xs
        v_new = u_i - jnp.einsum('bhck,bhkv->bhcv', w_i, state)
        o_i = jnp.einsum('bhck,bhkv->bhcv', qg_i, state) + jnp.einsum('bhcj,bhjv->bhcv', attn_i, v_new)
        state = state * jnp.exp(gl_i)[..., None, None] + jnp.einsum('bhck,bhcv->bhkv', kd_i, v_new)
        return state, o_i

    xs = tuple(jnp.moveaxis(t, 2, 0) for t in (qg, kd, u, w, attn, g_last))
    state0 = jnp.zeros((b, h, dk, dv), jnp.float32)
    _, o = lax.scan(step, state0, xs)
    return jnp.moveaxis(o, 0, 2).reshape(b, h, s, dv)


def _stick_breaking_attention(q, k, v):
    s_len, dh = q.shape[2], q.shape[3]
    scale = dh ** -0.5
    outs = []
    for blk in range(s_len // SB_QBLOCK):
        q0 = blk * SB_QBLOCK
        q1 = q0 + SB_QBLOCK
        kb, vb = k[:, :, :q1], v[:, :, :q1]
        z = jnp.einsum('bhqd,bhkd->bhqk', q[:, :, q0:q1], kb).astype(jnp.float32) * scale
        query_pos = q0 + jnp.arange(SB_QBLOCK)
        key_pos = jnp.arange(q1)
        causal = key_pos[None, :] < query_pos[:, None]
        log_stay = jnp.where(causal, jax.nn.log_sigmoid(-z), 0.0)
        log_stay_after = lax.cumsum(log_stay, axis=3, reverse=True) - log_stay
        weights = jnp.where(causal, jnp.exp(jax.nn.log_sigmoid(z) + log_stay_after), 0.0)
        outs.append(jnp.einsum('bhqk,bhkd->bhqd', weights.astype(vb.dtype), vb))
    return jnp.concatenate(outs, axis=2)


def _memory_attention(q, k, v):
    scores = jnp.einsum('bhqd,bhmd->bhqm', q, k).astype(jnp.float32) * (q.shape[-1] ** -0.5)
    p = jax.nn.softmax(scores, axis=-1)
    return jnp.einsum('bhqm,bhmd->bhqd', p.astype(v.dtype), v)


def _moe(x, w_router, b_router, w_gate_up, b_gate_up, w_down, b_down):
    b, s, d = x.shape
    t = b * s
    xt = x.reshape(t, d)
    logits = (xt @ w_router).astype(jnp.float32) + b_router.astype(jnp.float32)
    top_val, top_idx = lax.top_k(logits, TOP_K)
    gate = jax.nn.softmax(top_val, axis=-1)
    n_assign = t * TOP_K
    expert_flat = top_idx.reshape(n_assign)
    token_flat = jnp.repeat(jnp.arange(t, dtype=jnp.int32), TOP_K)
    gate_flat = gate.reshape(n_assign)
    order = jnp.argsort(expert_flat)
    e_sorted = expert_flat[order]
    t_sorted = token_flat[order]
    g_sorted = gate_flat[order]
    counts = jnp.bincount(expert_flat, length=N_EXPERTS)
    padded = (counts + MOE_BLOCK - 1) // MOE_BLOCK * MOE_BLOCK
    group_start = jnp.cumsum(counts) - counts
    padded_end = jnp.cumsum(padded)
    padded_start = padded_end - padded
    dst = padded_start[e_sorted] + jnp.arange(n_assign, dtype=jnp.int32) - group_start[e_sorted]
    n_blocks = -(-n_assign // MOE_BLOCK) + N_EXPERTS
    p_len = n_blocks * MOE_BLOCK
    tok_pad = jnp.full((p_len,), t, jnp.int32).at[dst].set(t_sorted)
    gate_pad = jnp.zeros((p_len,), jnp.float32).at[dst].set(g_sorted)
    block_start = jnp.arange(n_blocks, dtype=jnp.int32) * MOE_BLOCK
    block_expert = jnp.minimum(jnp.searchsorted(padded_end, block_start, side='right'), N_EXPERTS - 1)
    x_pad = jnp.concatenate([xt, jnp.zeros((1, d), xt.dtype)], axis=0)[tok_pad].reshape(n_blocks, MOE_BLOCK, d)

    def expert_block(args):
        xb, e = args
        hid = xb @ w_gate_up[e] + b_gate_up[e]
        glu = jnp.minimum(hid[:, :D_FF], SWIGLU_LIMIT)
        lin = jnp.clip(hid[:, D_FF:], -SWIGLU_LIMIT, SWIGLU_LIMIT)
        act = glu * jax.nn.sigmoid(SWIGLU_ALPHA * glu) * (lin + 1.0)
        return act @ w_down[e] + b_down[e]

    y_pad = lax.map(expert_block, (x_pad, block_expert)).reshape(p_len, d)
    y = jnp.zeros((t + 1, d), jnp.float32).at[tok_pad].add(y_pad.astype(jnp.float32) * gate_pad[:, None])[:t]
    return y.astype(x.dtype).reshape(b, s, d)


def setup_inputs(seed: int = 0) -> dict:
    key = jax.random.key(seed)
    ks = jax.random.split(key, 24)
    f32 = jnp.float32
    nrm = lambda k, shape, scale: jax.random.normal(k, shape, f32) * scale
    dt = jnp.exp(jax.random.uniform(ks[5], (DEPTH, GDN_HEADS), f32, np.log(1e-3), np.log(1e-1)))
    return {
        'x': nrm(ks[0], (BATCH, SEQ, D_MODEL), 1.0),
        'mem': nrm(ks[1], (BATCH, MEM_LEN, D_MODEL), 1.0),
        'w_in': nrm(ks[2], (DEPTH, D_MODEL, D_IN), D_MODEL ** -0.5),
        'w_conv': nrm(ks[3], (DEPTH, GDN_CONV, GDN_CONV_CH), GDN_CONV ** -0.5),
        'a_log': jnp.log(jax.random.uniform(ks[4], (DEPTH, GDN_HEADS), f32, 1.0, 16.0)),
        'dt_bias': dt + jnp.log(-jnp.expm1(-dt)),
        'gdn_norm_w': 1.0 + nrm(ks[6], (DEPTH, GDN_DV), 0.01),
        'w_mem_kv': nrm(ks[7], (DEPTH, D_MODEL, 2 * MEM_W), D_MODEL ** -0.5),
        'w_p_gdn': nrm(ks[8], (DEPTH, GDN_V, D_MODEL), GDN_V ** -0.5),
        'w_p_sb': nrm(ks[9], (DEPTH, SB_W, D_MODEL), SB_W ** -0.5),
        'w_p_mem': nrm(ks[10], (DEPTH, MEM_W, D_MODEL), MEM_W ** -0.5),
        'w_o': nrm(ks[11], (DEPTH, D_MODEL, D_MODEL), D_MODEL ** -0.5 * DEEPNORM_BETA),
        'ln1_g': 1.0 + nrm(ks[12], (DEPTH, D_MODEL), 0.01),
        'ln1_b': nrm(ks[13], (DEPTH, D_MODEL), 0.01),
        'w_router': nrm(ks[14], (DEPTH, D_MODEL, N_EXPERTS), D_MODEL ** -0.5),
        'b_router': nrm(ks[15], (DEPTH, N_EXPERTS), 0.01),
        'w_gate_up': nrm(ks[16], (DEPTH, N_EXPERTS, D_MODEL, 2 * D_FF), D_MODEL ** -0.5),
        'b_gate_up': nrm(ks[17], (DEPTH, N_EXPERTS, 2 * D_FF), 0.01),
        'w_down': nrm(ks[18], (DEPTH, N_EXPERTS, D_FF, D_MODEL), D_FF ** -0.5 * DEEPNORM_BETA),
        'b_down': nrm(ks[19], (DEPTH, N_EXPERTS, D_MODEL), 0.01),
        'ln2_g': 1.0 + nrm(ks[20], (DEPTH, D_MODEL), 0.01),
        'ln2_b': nrm(ks[21], (DEPTH, D_MODEL), 0.01),
    }


def reference(x, mem, w_in, w_conv, a_log, dt_bias, gdn_norm_w, w_mem_kv, w_p_gdn, w_p_sb, w_p_mem, w_o,
              ln1_g, ln1_b, w_router, b_router, w_gate_up, b_gate_up, w_down, b_down, ln2_g, ln2_b):
    b, s, d = x.shape
    split_idx = np.cumsum(IN_SIZES)[:-1].tolist()
    for l in range(DEPTH):
        proj = x @ w_in[l]
        gq, gk, gv, gz, ga, gb, sq, sk, sv, mq, gates = jnp.split(proj, split_idx, axis=-1)

        qkv = jax.nn.silu(_causal_depthwise_conv(jnp.concatenate([gq, gk, gv], axis=-1), w_conv[l]))
        cq, ck, cv = jnp.split(qkv, [GDN_QK, 2 * GDN_QK], axis=-1)
        q = _l2norm(_heads(cq, GDN_HEADS).astype(jnp.float32)) * (GDN_DK ** -0.5)
        k = _l2norm(_heads(ck, GDN_HEADS).astype(jnp.float32))
        v = _heads(cv, GDN_HEADS).astype(jnp.float32)
        log_decay = -jnp.exp(a_log[l]) * jax.nn.softplus(ga.astype(jnp.float32) + dt_bias[l])
        beta = jax.nn.sigmoid(gb.astype(jnp.float32))
        o = _gated_delta_rule(q, k, v, log_decay.transpose(0, 2, 1), beta.transpose(0, 2, 1))
        o = o * lax.rsqrt(jnp.mean(o * o, -1, keepdims=True) + NORM_EPS) * gdn_norm_w[l].astype(jnp.float32)
        z = gz.reshape(b, s, GDN_HEADS, GDN_DV).astype(jnp.float32)
        y_gdn = (o.transpose(0, 2, 1, 3) * jax.nn.silu(z)).reshape(b, s, GDN_V).astype(x.dtype)

        y_sb = _stick_breaking_attention(_heads(sq, SB_HEADS), _heads(sk, SB_HEADS), _heads(sv, SB_HEADS))
        y_sb = y_sb.transpose(0, 2, 1, 3).reshape(b, s, SB_W)

        mk, mv = jnp.split(mem @ w_mem_kv[l], 2, axis=-1)
        y_mem = _memory_attention(_heads(mq, MEM_HEADS), _heads(mk, MEM_HEADS), _heads(mv, MEM_HEADS))
        y_mem = y_mem.transpose(0, 2, 1, 3).reshape(b, s, MEM_W)

        g = jax.nn.sigmoid(gates.reshape(b, s, N_BRANCHES, d))
        mixed = (g[:, :, 0] * (y_gdn @ w_p_gdn[l]) + g[:, :, 1] * (y_sb @ w_p_sb[l])
                 + g[:, :, 2] * (y_mem @ w_p_mem[l]))
        x = _layer_norm(DEEPNORM_ALPHA * x + mixed @ w_o[l], ln1_g[l], ln1_b[l])

        y_moe = _moe(x, w_router[l], b_router[l], w_gate_up[l], b_gate_up[l], w_down[l], b_down[l])
        x = _layer_norm(DEEPNORM_ALPHA * x + y_moe, ln2_g[l], ln2_b[l])
    return x
```

```python
import numpy as np
import concourse.bass as bass
import concourse.mybir as mybir
from concourse.bass_utils import run_bass_kernel_spmd
from contextlib import ExitStack

F32 = mybir.dt.float32
F32R = mybir.dt.float32r
BF16 = mybir.dt.bfloat16
AF = mybir.ActivationFunctionType
ALU = mybir.AluOpType
AX = mybir.AxisListType

NSEQ = 2
S = 2048
D = 1024
TT = 256
NTT = S // TT
G = 1024
NEXP = 32
ALPHA = 2.0 ** 0.25
ARENA = 52000


class Tk:
    __slots__ = ("name", "w", "r")

    def __init__(self, name=""):
        self.name = name
        self.w = None
        self.r = []


class Op:
    __slots__ = ("eng", "fn", "deps", "need_inc", "sem", "val", "is_dma")


class Slot:
    def __init__(self, prog, name):
        self.sem = prog.es.enter_context(prog.nc.semaphore("d_" + name))
        self.count = 0
        self.tk = Tk(name)
        self.ap = None


class Prog:
    ENGS = ["pe", "act", "dve", "pool", "sp"]

    def __init__(self, nc, es):
        self.nc = nc
        self.es = es
        self.ops = {e: [] for e in self.ENGS}
        self.engsem = {e: es.enter_context(nc.semaphore("s_" + e)) for e in self.ENGS}
        self.dmas = []
        self.dmas_all = []

    def slot(self, name):
        return Slot(self, name)

    def _record(self, eng, fn, reads, writes, is_dma=False, slot=None, extra_deps=()):
        op = Op()
        op.eng = eng
        op.fn = fn
        op.is_dma = is_dma
        op.need_inc = False
        deps = list(extra_deps)
        for t in reads:
            if t.w is not None:
                deps.append(t.w)
        for t in writes:
            if t.w is not None:
                deps.append(t.w)
            deps.extend(t.r)
        fdeps = []
        for d in deps:
            if (not d.is_dma) and (not is_dma) and d.eng == eng and eng == "pe":
                continue
            fdeps.append(d)
        op.deps = fdeps
        if is_dma:
            slot.count += 16
            op.sem = slot.sem
            op.val = slot.count
            self.dmas.append(op)
            self.dmas_all.append(op)
        else:
            op.sem = None
            op.val = None
        for t in writes:
            t.w = op
            t.r = []
        for t in reads:
            if t in writes:
                continue
            if is_dma:
                t.r.append(op)
            else:
                t.r = [x for x in t.r if x.is_dma or x.eng != eng]
                t.r.append(op)
        self.ops[eng].append(op)
        return op

    def call(self, eng, meth, reads, writes, *a, **kw):
        def fn(e):
            return getattr(e, meth)(*a, **kw)
        return self._record(eng, fn, list(reads), list(writes))

    def mm(self, out, lhsT, rhs, start, stop, reads, writes):
        def fn(e):
            return e.matmul(out, lhsT, rhs, start=start, stop=stop)
        return self._record("pe", fn, list(reads), list(writes))

    def tr(self, out, in_, ident, reads, writes):
        def fn(e):
            return e.transpose(out, in_, ident)
        return self._record("pe", fn, list(reads), list(writes))

    def dma(self, q, out, in_, slot, reads=(), writes=()):
        def fn(e):
            return e.dma_start(out=out, in_=in_)
        return self._record(q, fn, list(reads), list(writes), is_dma=True, slot=slot)

    def dma_in(self, q, out, in_, slot):
        if in_.dtype != out.dtype:
            out = out.bitcast(in_.dtype)
        return self.dma(q, out, in_, slot, reads=(), writes=(slot.tk,))

    def barrier(self):
        lasts = []
        for e in self.ENGS:
            for op in reversed(self.ops[e]):
                if not op.is_dma and op.fn is not None:
                    lasts.append(op)
                    break
        deps = lasts + list(self.dmas)
        self.dmas = []
        for e in self.ENGS:
            self._record(e, None, [], [], extra_deps=deps)

    def emit(self, final_waits=()):
        nc = self.nc
        for e in self.ENGS:
            for op in self.ops[e]:
                for d in op.deps:
                    if not d.is_dma:
                        d.need_inc = True
        for e in self.ENGS:
            c = 0
            for op in self.ops[e]:
                if op.is_dma:
                    continue
                if op.need_inc:
                    assert op.fn is not None
                    c += 1
                    op.sem = self.engsem[e]
                    op.val = c
        self.stats = {e: len(self.ops[e]) for e in self.ENGS}
        self.stats["maxsem"] = {e: max([op.val for op in self.ops[e] if (not op.is_dma) and op.val] + [0]) for e in self.ENGS}
        self.stats["maxdma"] = max([op.val for op in self.dmas_all] + [0])

        def run(eng_name, e):
            waited = {}
            for op in self.ops[eng_name]:
                need = {}
                for d in op.deps:
                    k = id(d.sem)
                    if k not in need or need[k][1] < d.val:
                        need[k] = (d.sem, d.val)
                for k, (sem, val) in need.items():
                    if waited.get(k, 0) >= val:
                        continue
                    e.wait_ge(sem, val)
                    waited[k] = val
                if op.fn is None:
                    continue
                ins = op.fn(e)
                if op.is_dma:
                    ins.then_inc(op.sem, 16)
                elif op.need_inc:
                    ins.then_inc(op.sem, 1)
            if eng_name == "sp":
                for d in final_waits:
                    e.wait_ge(d.sem, d.val)

        with nc.Block() as block:
            @block.tensor
            def _(e):
                run("pe", e)

            @block.scalar
            def _(e):
                run("act", e)

            @block.vector
            def _(e):
                run("dve", e)

            @block.gpsimd
            def _(e):
                run("pool", e)

            @block.sync
            def _(e):
                run("sp", e)


def R(ap):
    return ap.bitcast(F32R)


class Buf:
    def __init__(self, ap, name=""):
        self.ap = ap
        self.tk = Tk(name)


def build_program(nseq=NSEQ, ntt=NTT, phase_b=True, nexp=NEXP, dbg=False, skip=(), phase_a=True, ngrp=None):
    nc = bass.Bass("TRN2", target_bir_lowering=False)
    nc.dge_precook = False

    def din(name, shape, dt=F32):
        return nc.dram_tensor(name, list(shape), dt, kind="ExternalInput").ap()

    xT_d = din("xT", [NSEQ, 1024, S], F32R)
    x_d = din("x", [NSEQ, S, D])
    memT_d = din("memT", [NSEQ, 1024, 256], F32R)
    wfm_d = din("wfm", [28, 128, 2048], F32R)
    wab_d = din("wab", [128, 64])
    wconv_d = din("wconv", [128, 48])
    alog_d = din("alog", [128, 4])
    dtb_d = din("dtb", [128, 4])
    gnw_d = din("gnw", [128, 1])
    wmkv_d = din("wmkv", [4, 128, 2048], F32R)
    wp_d = din("wp", [24, 128, 512], F32R)
    wo_d = din("wo", [4, 128, 2048], F32R)
    ln_d = din("ln", [4, 128, 1024])
    wr_d = din("wr", [128, 256])
    br_d = din("br", [128, 32])
    wgu_d = din("wgu", [256 if phase_b else 1, 128, 2048], F32R)
    bgu_d = din("bgu", [128, 512])
    wd_d = din("wd", [32 if phase_b else 1, 128, 8192], F32R)
    bd_d = din("bd", [32, 1024])
    x1_d = (nc.dram_tensor("x1s", [NSEQ, S, D], F32, kind=("ExternalOutput" if phase_a else "ExternalInput")).ap() if dbg else
            nc.dram_tensor("x1s", [NSEQ, S, D], F32).ap())
    out_d = nc.dram_tensor("out", [NSEQ, S, D], F32, kind="ExternalOutput").ap()
    dbg_d = nc.dram_tensor("dbg_ybr", [3, 512, S], F32, kind="ExternalOutput").ap() if dbg else None

    with ExitStack() as es:
        p = Prog(nc, es)
        sb_base = (nc.sbuf_base + 31) // 32 * 32
        assert sb_base + ARENA * 4 <= nc.sbuf_top, (nc.sbuf_base, nc.sbuf_top)
        nalloc = [0]
        psum_t = es.enter_context(nc.psum_tensor("psum", [128, 4096], F32))
        aoff = [0]

        def alloc(cols, name=""):
            a = aoff[0]
            aoff[0] += (cols + 7) // 8 * 8
            assert aoff[0] <= ARENA, (name, aoff[0])
            nalloc[0] += 1
            t = nc.alloc_sbuf_tensor_at("%s_%d" % (name, nalloc[0]), [128, cols], F32, offset=sb_base + a * 4)
            return Buf(t[:], name)

        def alloc_slot(cols, name):
            b = alloc(cols, name)
            sl = p.slot(name)
            sl.ap = b.ap
            return sl

        banks = [Buf(psum_t[:, i * 512:(i + 1) * 512], "bank%d" % i) for i in range(8)]
        bank_rr = {"i": 0, "lo": 4, "n": 3}

        def newbank():
            b = banks[bank_rr["lo"] + bank_rr["i"] % bank_rr["n"]]
            bank_rr["i"] += 1
            return b

        ident = alloc(128, "ident")
        ones = alloc(128, "ones")
        Lm = alloc(128, "L")
        mstrict = alloc(128, "mstrict")
        mbincl = alloc(128, "mbincl")
        cm01 = alloc(128, "cm01")
        identb = alloc(64, "identb")
        cm01b = alloc(64, "cm01b")
        identb_ap = identb.ap.bitcast(BF16)
        cm01b_ap = cm01b.ap.bitcast(BF16)
        CONST_END = None

        p.call("pool", "memset", [], [ones.tk], ones.ap, 1.0)
        p.call("pool", "affine_select", [ones.tk], [ident.tk], out=ident.ap, in_=ones.ap, pattern=[[1, 128]],
               compare_op=ALU.is_equal, fill=0.0, base=0, channel_multiplier=-1)
        p.call("pool", "affine_select", [ones.tk], [Lm.tk], out=Lm.ap, in_=ones.ap, pattern=[[1, 128]],
               compare_op=ALU.is_ge, fill=0.0, base=0, channel_multiplier=-1)
        p.call("pool", "memset", [], [Lm.tk], Lm.ap[0:64, 64:128], 0.0)
        p.call("pool", "affine_select", [ones.tk], [mstrict.tk], out=mstrict.ap, in_=ones.ap, pattern=[[1, 128]],
               compare_op=ALU.is_ge, fill=0.0, base=-1, channel_multiplier=-1)
        p.call("pool", "memset", [], [mstrict.tk], mstrict.ap[0:64, 64:128], 0.0)
        p.call("dve", "tensor_scalar", [Lm.tk], [mbincl.tk], out=mbincl.ap, in0=Lm.ap, scalar1=-1.0, scalar2=1e30,
               op0=ALU.add, op1=ALU.mult)
        p.call("pool", "affine_select", [ones.tk], [cm01.tk], out=cm01.ap, in_=ones.ap, pattern=[[-1, 128]],
               compare_op=ALU.is_ge, fill=0.0, base=-1, channel_multiplier=1)
        p.call("dve", "tensor_copy", [cm01.tk], [cm01b.tk], out=cm01b_ap, in_=cm01.ap)
        p.call("dve", "tensor_copy", [ident.tk], [identb.tk], out=identb_ap, in_=ident.ap)
        cst = alloc(4, "cst")
        p.call("pool", "memset", [], [cst.tk], cst.ap[:, 0:1], 0.0)
        p.call("pool", "memset", [], [cst.tk], cst.ap[:, 1:2], 7.0)
        pmask = alloc(2, "pmask")
        p.call("pool", "memset", [], [pmask.tk], pmask.ap, 0.0)
        p.call("pool", "memset", [], [pmask.tk], pmask.ap[0:64, 0:1], 1.0)
        p.call("pool", "memset", [], [pmask.tk], pmask.ap[64:128, 1:2], 1.0)
        base_off = aoff[0]

        sl_wconv = alloc_slot(48, "wconv")
        sl_alog = alloc_slot(4, "alog")
        sl_dtb = alloc_slot(4, "dtb")
        sl_gnw = alloc_slot(1, "gnw")
        sl_wab = alloc_slot(64, "wab")
        sl_ln1g = alloc_slot(1024, "ln1g")
        sl_ln1b = alloc_slot(1024, "ln1b")
        for sl, src in ((sl_wconv, wconv_d), (sl_alog, alog_d), (sl_dtb, dtb_d), (sl_gnw, gnw_d), (sl_wab, wab_d),
                        (sl_ln1g, ln_d[0]), (sl_ln1b, ln_d[1])):
            p.dma_in("sp", sl.ap, src, sl)
        negA = alloc(4, "negA")
        p.call("act", "activation", [sl_alog.tk], [negA.tk], out=negA.ap, in_=sl_alog.ap, func=AF.Exp)
        p.call("dve", "tensor_scalar", [negA.tk], [negA.tk], out=negA.ap, in0=negA.ap, scalar1=-1.0, scalar2=None,
               op0=ALU.mult)

        KT = alloc(4096, "KT")
        KT_ap = KT.ap.bitcast(BF16).rearrange("p (a n) -> p a n", a=4)
        KT_tk = [Tk("KT%d" % i) for i in range(NTT)]
        Vb = alloc(4096, "V")
        V_ap = Vb.ap.bitcast(BF16).rearrange("p (a n) -> p a n", a=16)
        V_tk = [Tk("V%d" % i) for i in range(16)]
        Sst = alloc(512, "S")
        S_ap = Sst.ap.rearrange("p (h n) -> p h n", h=4)
        S_tk = [Tk("S%d" % h) for h in range(4)]
        memKT = alloc(1024, "memKT")
        memKT_ap = memKT.ap.rearrange("p (h n) -> p h n", h=4)
        memV = alloc(1024, "memV")
        memV_ap = memV.ap.rearrange("p (a n) -> p a n", a=2)
        carry = alloc(36, "carry")
        carry_ap = carry.ap.rearrange("p (c n) -> p c n", c=12)
        carry_tk = [Tk("carry%d" % i) for i in range(12)]

        sl_xT = alloc_slot(8 * TT, "xT")
        xT_ap = sl_xT.ap.rearrange("p (k n) -> p k n", k=8)
        ring = [alloc_slot(2048, "ring%d" % i) for i in range(3)]
        ring_i = [0]

        def ring_next():
            sl = ring[ring_i[0] % 3]
            ring_i[0] += 1
            return sl

        wpring = [alloc_slot(512, "wpr%d" % i) for i in range(3)]
        wpr_i = [0]
        sl_x = [alloc_slot(1024, "xtile%d" % i) for i in range(2)]
        pc = alloc(3 + TT, "pc")
        acc = alloc(TT, "acc")
        sqb = alloc(TT, "sqb")
        rnb = alloc(TT, "rnb")
        qT = [alloc(TT, "qT%d" % h) for h in range(4)]
        kT = [alloc(TT, "kT%d" % h) for h in range(4)]
        vT = [alloc(TT, "vT%d" % h) for h in range(4)]
        zsT = [alloc(TT, "zsT%d" % h) for h in range(4)]
        sqT = alloc(4 * TT // 2, "sqT")
        sqT_ap = sqT.ap.bitcast(BF16).rearrange("p (a n) -> p a n", a=4)
        mqT = alloc(4 * TT, "mqT")
        mqT_ap = mqT.ap.rearrange("p (a n) -> p a n", a=4)
        svT = alloc(TT, "svT")
        ybr = [alloc(4 * TT, "ybr%d" % i) for i in range(3)]
        ybr_ap = [b.ap.rearrange("p (a n) -> p a n", a=4) for b in ybr]
        mixedT = alloc(8 * TT, "mixedT")
        mixedT_ap = mixedT.ap.rearrange("p (a n) -> p a n", a=8)
        mixtmp = alloc(TT, "mixtmp")
        gsig = alloc(TT, "gsig")
        rbuf = [alloc_slot(1024, "r%d" % i) for i in range(2)]
        junk = alloc(1024, "junk")
        abt = alloc(16, "abt")
        gsm = alloc(64, "gsmall")
        lnst = alloc(16, "lnst")
        gd = {n: alloc(128, "gd_" + n) for n in
              ["Gb", "egcb", "Dm", "dinc", "dstr", "X", "XT", "Y", "YT", "Y2", "YT2", "Rm", "attnT", "ktok", "vtok",
               "kd", "kd1", "qg", "nr", "vnew"]}
        kds = alloc(3, "kds")
        otok = alloc(512, "otok")
        osq = alloc(4, "osq")
        mP = alloc(256, "mP")
        mPT = alloc(256, "mPT")
        msm = alloc(8, "msm")
        sbsp = alloc(2048, "sbsp")
        sbC = alloc(2048, "sbC")
        sbW = alloc(2048, "sbW")
        sbW_ap = sbW.ap
        sbWT = [alloc(256, "sbWT%d" % i) for i in range(2)]
        sbsm = alloc(4, "sbsm")
        print("phase A arena use", aoff[0])

        def evac(eng, dst_ap, dst_tks, src_ap, src_tks, **kw):
            if eng == "act":
                p.call("act", "activation", src_tks, dst_tks, out=dst_ap, in_=src_ap, func=AF.Copy, **kw)
            else:
                p.call(eng, "tensor_copy", src_tks, dst_tks, out=dst_ap, in_=src_ap)

        def layer_norm(rb, g_sl, b_sl, out_ap, out_tks):
            st = lnst.ap
            p.call("act", "activation", [rb.tk], [junk.tk, lnst.tk], out=junk.ap, in_=rb.ap, func=AF.Identity,
                   accum_out=st[:, 0:1])
            p.call("act", "activation", [rb.tk], [junk.tk, lnst.tk], out=junk.ap, in_=rb.ap, func=AF.Square,
                   accum_out=st[:, 1:2])
            p.call("dve", "tensor_scalar", [lnst.tk], [lnst.tk], out=st[:, 2:4], in0=st[:, 0:2], scalar1=1.0 / D,
                   scalar2=None, op0=ALU.mult)
            p.call("dve", "tensor_tensor", [lnst.tk], [lnst.tk], out=st[:, 4:5], in0=st[:, 2:3], in1=st[:, 2:3],
                   op=ALU.mult)
            p.call("dve", "tensor_tensor", [lnst.tk], [lnst.tk], out=st[:, 5:6], in0=st[:, 3:4], in1=st[:, 4:5],
                   op=ALU.subtract)
            p.call("act", "activation", [lnst.tk], [lnst.tk], out=st[:, 6:7], in_=st[:, 5:6], func=AF.Sqrt,
                   bias=st[:, 8:9], scale=1.0)
            p.call("dve", "reciprocal", [lnst.tk], [lnst.tk], out=st[:, 7:8], in_=st[:, 6:7])
            p.call("dve", "tensor_scalar", [rb.tk, lnst.tk], [rb.tk], out=rb.ap, in0=rb.ap, scalar1=st[:, 2:3],
                   scalar2=st[:, 7:8], op0=ALU.subtract, op1=ALU.mult)
            p.call("pool", "tensor_tensor", [rb.tk, g_sl.tk], [rb.tk], out=rb.ap, in0=rb.ap, in1=g_sl.ap,
                   op=ALU.mult)
            p.call("dve", "tensor_tensor", [rb.tk, b_sl.tk], out_tks, out=out_ap, in0=rb.ap, in1=b_sl.ap,
                   op=ALU.add)

        p.call("pool", "memset", [], [lnst.tk], lnst.ap[:, 8:9], 1e-5)
        p.call("pool", "memset", [], [lnst.tk], lnst.ap[:, 9:10], 1e-6)

        x1o_i = [0]

        x1_stores = []
        for s in range(nseq if phase_a else 0):
            p.call("pool", "memset", [], S_tk, Sst.ap, 0.0)
            p.call("pool", "memset", [], carry_tk, carry.ap, 0.0)
            p.call("pool", "memset", [], [gd["nr"].tk], gd["nr"].ap, 0.0)
            p.call("pool", "memset", [], [gd["vnew"].tk], gd["vnew"].ap, 0.0)
            if dbg:
                for bb_ in ybr + [mixedT]:
                    ncol_ = bb_.ap.shape[-1]
                    p.call("dve", "tensor_scalar", [ones.tk], [bb_.tk], out=R(bb_.ap),
                           in0=ones.ap[:, 0:1].to_broadcast([128, ncol_]), scalar1=0.0, scalar2=None, op0=ALU.mult)
            memT_ap = sl_xT.ap.rearrange("p (k n) -> p k n", k=8)
            p.dma_in("sp", memT_ap, memT_d[s].rearrange("(k p) n -> p k n", p=128), sl_xT)
            for i in range(4):
                sl = ring_next()
                p.dma_in("sp", sl.ap, wmkv_d[i], sl)
                w3 = sl.ap.rearrange("p (k c) -> p k c", k=8)
                if i < 2:
                    for c2 in range(2):
                        h = 2 * i + c2
                        b = newbank()
                        for kc in range(8):
                            p.mm(b.ap[:, 0:256], R(w3[:, kc, c2 * 128:(c2 + 1) * 128]), R(memT_ap[:, kc, :]),
                                 kc == 0, kc == 7, [sl.tk, sl_xT.tk], [b.tk])
                        evac("act", memKT_ap[:, h, :], [memKT.tk], b.ap[:, 0:256], [b.tk])
                else:
                    for mt in range(2):
                        b = newbank()
                        for kc in range(8):
                            p.mm(b.ap[:, 0:256], R(memT_ap[:, kc, mt * 128:(mt + 1) * 128]), R(w3[:, kc, :]),
                                 kc == 0, kc == 7, [sl.tk, sl_xT.tk], [b.tk])
                        evac("dve", memV_ap[:, mt, (i - 2) * 256:(i - 1) * 256], [memV.tk], b.ap[:, 0:256], [b.tk])

            for tt in range(ntt):
                t0 = tt * TT
                p.dma_in("sp", xT_ap, xT_d[s].rearrange("(k p) n -> p k n", p=128)[:, :, t0:t0 + TT], sl_xT)
                for sub in range(2):
                    p.dma_in("sp", sl_x[sub].ap, x_d[s, t0 + sub * 128:t0 + (sub + 1) * 128, :], sl_x[sub])

                def l2norm(dst, scale):
                    p.call("act", "activation", [dst.tk], [sqb.tk], out=sqb.ap, in_=dst.ap, func=AF.Square)
                    b = newbank()
                    p.mm(b.ap[:, 0:TT], ones.ap, sqb.ap, True, True, [ones.tk, sqb.tk], [b.tk])
                    p.call("act", "activation", [b.tk, lnst.tk], [rnb.tk], out=rnb.ap, in_=b.ap[:, 0:TT], func=AF.Sqrt,
                           bias=lnst.ap[:, 9:10], scale=1.0)
                    p.call("dve", "reciprocal", [rnb.tk], [rnb.tk], out=rnb.ap, in_=rnb.ap)
                    p.call("dve", "scalar_tensor_tensor", [dst.tk, rnb.tk], [dst.tk], out=dst.ap, in0=dst.ap,
                           scalar=scale, in1=rnb.ap, op0=ALU.mult, op1=ALU.mult)

                def h_gdn(kind, h, bsrc):
                    ch = kind * 4 + h
                    wc = sl_wconv.ap
                    p.call("act", "activation", [bsrc.tk], [pc.tk], out=pc.ap[:, 3:3 + TT], in_=bsrc.ap[:, 0:TT],
                           func=AF.Copy)
                    p.call("dve", "tensor_copy", [carry_tk[ch]], [pc.tk], out=pc.ap[:, 0:3], in_=carry_ap[:, ch, :])
                    p.call("dve", "tensor_copy", [pc.tk], [carry_tk[ch]], out=carry_ap[:, ch, :],
                           in_=pc.ap[:, TT:TT + 3])
                    p.call("dve", "tensor_scalar", [pc.tk, sl_wconv.tk], [acc.tk], out=acc.ap, in0=pc.ap[:, 0:TT],
                           scalar1=wc[:, ch * 4:ch * 4 + 1], scalar2=None, op0=ALU.mult)
                    for j in range(1, 4):
                        p.call("dve", "scalar_tensor_tensor", [pc.tk, acc.tk, sl_wconv.tk], [acc.tk], out=acc.ap,
                               in0=pc.ap[:, j:j + TT], scalar=wc[:, ch * 4 + j:ch * 4 + j + 1], in1=acc.ap,
                               op0=ALU.mult, op1=ALU.add)
                    dst = (qT, kT, vT)[kind][h]
                    p.call("act", "activation", [acc.tk], [dst.tk], out=dst.ap, in_=acc.ap, func=AF.Silu)
                    if kind == 0:
                        l2norm(dst, 128.0 ** -0.5)
                    elif kind == 1:
                        l2norm(dst, 1.0)

                def handler(pi, c2, b):
                    src = b.ap[:, 0:TT]
                    if pi < 6:
                        h_gdn(pi // 2, 2 * (pi % 2) + c2, b)
                    elif pi < 8:
                        h = 2 * (pi - 6) + c2
                        p.call("act", "activation", [b.tk], [zsT[h].tk], out=zsT[h].ap, in_=src, func=AF.Silu)
                    elif pi < 10:
                        pr = 2 * (pi - 8) + c2
                        p.call("act", "activation", [b.tk], [sqT.tk], out=sqT_ap[:, pr, :], in_=src, func=AF.Copy,
                               scale=0.125)
                    elif pi < 12:
                        pr = 2 * (pi - 10) + c2
                        p.call("dve", "tensor_copy", [b.tk], [KT_tk[tt]], out=KT_ap[:, pr, t0:t0 + TT], in_=src)
                    elif pi < 14:
                        pr = 2 * (pi - 12) + c2
                        evac("act", svT.ap, [svT.tk], src, [b.tk])
                        for sub in range(2):
                            b2 = newbank()
                            p.tr(b2.ap[:, 0:128], svT.ap[:, sub * 128:(sub + 1) * 128], ident.ap, [svT.tk, ident.tk],
                                 [b2.tk])
                            p.call("dve", "tensor_copy", [b2.tk], [V_tk[tt * 2 + sub]],
                                   out=V_ap[:, tt * 2 + sub, pr * 128:(pr + 1) * 128], in_=b2.ap[:, 0:128])
                    else:
                        h = 2 * (pi - 14) + c2
                        p.call("act", "activation", [b.tk], [mqT.tk], out=mqT_ap[:, h, :], in_=src, func=AF.Copy,
                               scale=128.0 ** -0.5)

                def inproj_pair(pi, hd):
                    sl = ring_next()
                    p.dma_in("sp", sl.ap, wfm_d[pi], sl)
                    w3 = sl.ap.rearrange("p (k c) -> p k c", k=8)
                    for c2 in range(2):
                        b = newbank()
                        for kc in range(8):
                            p.mm(b.ap[:, 0:TT], R(w3[:, kc, c2 * 128:(c2 + 1) * 128]), R(xT_ap[:, kc, :]),
                                 kc == 0, kc == 7, [sl.tk, sl_xT.tk], [b.tk])
                        hd(pi, c2, b)

                for pi in range(16):
                    inproj_pair(pi, handler)

                gs = gsm.ap
                for sub in range(2):
                    b = newbank()
                    for kc in range(8):
                        p.mm(b.ap[:, 0:8], xT_ap[:, kc, sub * 128:(sub + 1) * 128],
                             sl_wab.ap[:, kc * 8:(kc + 1) * 8], kc == 0, kc == 7, [sl_xT.tk, sl_wab.tk], [b.tk])
                    evac("dve", abt.ap[:, sub * 8:(sub + 1) * 8], [abt.tk], b.ap[:, 0:8], [b.tk])

                for sub in range(2):
                    c0 = sub * 128
                    p.barrier()
                    ab = abt.ap[:, sub * 8:(sub + 1) * 8]
                    p.call("dve", "tensor_tensor", [abt.tk, sl_dtb.tk], [gsm.tk], out=gs[:, 0:4], in0=ab[:, 0:4],
                           in1=sl_dtb.ap, op=ALU.add)
                    p.call("act", "activation", [gsm.tk], [gsm.tk], out=gs[:, 0:4], in_=gs[:, 0:4], func=AF.Exp)
                    p.call("act", "activation", [gsm.tk], [gsm.tk], out=gs[:, 0:4], in_=gs[:, 0:4], func=AF.Ln,
                           bias=1.0, scale=1.0)
                    p.call("dve", "tensor_tensor", [gsm.tk, negA.tk], [gsm.tk], out=gs[:, 4:8], in0=gs[:, 0:4],
                           in1=negA.ap, op=ALU.mult)
                    p.call("act", "activation", [abt.tk], [gsm.tk], out=gs[:, 8:12], in_=ab[:, 4:8], func=AF.Sigmoid)
                    p.call("dve", "tensor_scalar", [gsm.tk], [gsm.tk], out=gs[:, 12:16], in0=gs[:, 8:12], scalar1=-1.0,
                           scalar2=None, op0=ALU.mult)
                    b = newbank()
                    p.mm(b.ap[:, 0:4], Lm.ap, gs[:, 4:8], True, True, [Lm.tk, gsm.tk], [b.tk])
                    evac("dve", gs[:, 16:20], [gsm.tk], b.ap[:, 0:4], [b.tk])
                    p.call("act", "activation", [gsm.tk], [gsm.tk], out=gs[:, 20:24], in_=gs[:, 16:20], func=AF.Exp)

                    for h in (range(3, 4) if "gdnH3" in skip else range((1 if "gdnH1" in skip else 4) if "gdn" not in skip else 0)):
                        T = gd
                        p.barrier()
                        qs = qT[h].ap[:, c0:c0 + 128]
                        ks = kT[h].ap[:, c0:c0 + 128]
                        vs = vT[h].ap[:, c0:c0 + 128]
                        p.call("dve", "tensor_scalar", [ones.tk, gsm.tk], [T["Gb"].tk], out=T["Gb"].ap, in0=ones.ap,
                               scalar1=gs[:, 4 + h:5 + h], scalar2=None, op0=ALU.mult)
                        bg = newbank()
                        p.mm(bg.ap[:, 0:128], T["Gb"].ap, Lm.ap, True, True, [T["Gb"].tk, Lm.tk], [bg.tk])
                        p.call("act", "activation", [bg.tk], [T["egcb"].tk], out=T["egcb"].ap, in_=bg.ap[:, 0:128],
                               func=AF.Exp)
                        if "gdnP1a" in skip:
                            continue
                        p.call("dve", "tensor_scalar", [bg.tk, gsm.tk, cst.tk], [T["Dm"].tk], out=T["Dm"].ap,
                               in0=bg.ap[:, 0:128], scalar1=gs[:, 16 + h:17 + h], scalar2=cst.ap[:, 0:1],
                               op0=ALU.subtract, op1=ALU.min)
                        if "gdnP1a2" in skip:
                            continue
                        p.call("act", "activation", [T["Dm"].tk], [T["Dm"].tk], out=T["Dm"].ap, in_=T["Dm"].ap,
                               func=AF.Exp)
                        if "gdnP1a3" in skip:
                            continue
                        p.call("pool", "tensor_tensor", [T["Dm"].tk, Lm.tk], [T["dinc"].tk], out=T["dinc"].ap,
                               in0=T["Dm"].ap, in1=Lm.ap, op=ALU.mult)
                        if "gdnP1b" in skip:
                            continue
                        p.call("pool", "tensor_tensor", [T["dinc"].tk, mstrict.tk], [T["dstr"].tk], out=T["dstr"].ap,
                               in0=T["dinc"].ap, in1=mstrict.ap, op=ALU.mult)
                        if "gdnP1c" in skip:
                            continue
                        p.call("dve", "tensor_copy", [T["dinc"].tk], [kds.tk], out=kds.ap[0:64, 0:1],
                               in_=T["dinc"].ap[0:64, 63:64])
                        p.call("dve", "tensor_copy", [T["dinc"].tk], [kds.tk], out=kds.ap[64:128, 0:1],
                               in_=T["dinc"].ap[64:128, 127:128])
                        if "gdnP1" in skip:
                            continue
                        bk = newbank()
                        p.mm(bk.ap[:, 0:128], ks, ks, True, True, [kT[h].tk], [bk.tk])
                        p.call("dve", "scalar_tensor_tensor", [bk.tk, gsm.tk, T["dstr"].tk], [T["X"].tk],
                               out=T["X"].ap, in0=bk.ap[:, 0:128], scalar=gs[:, 8 + h:9 + h], in1=T["dstr"].ap,
                               op0=ALU.mult, op1=ALU.mult)
                        ba = newbank()
                        p.mm(ba.ap[:, 0:128], ks, qs, True, True, [kT[h].tk, qT[h].tk], [ba.tk])
                        p.call("dve", "tensor_tensor", [ba.tk, T["dinc"].tk], [T["attnT"].tk], out=T["attnT"].ap,
                               in0=ba.ap[:, 0:128], in1=T["dinc"].ap, op=ALU.mult)
                        bx = newbank()
                        p.tr(bx.ap[:, 0:128], T["X"].ap, ident.ap, [T["X"].tk, ident.tk], [bx.tk])
                        evac("act", T["XT"].ap, [T["XT"].tk], bx.ap[:, 0:128], [bx.tk])
                        p.call("pool", "tensor_tensor", [ident.tk, T["X"].tk], [T["Rm"].tk], out=T["Rm"].ap,
                               in0=ident.ap, in1=T["X"].ap, op=ALU.subtract)
                        if "gdnP2" in skip:
                            continue
                        Y, YT = T["X"], T["XT"]
                        nxt = [(T["Y"], T["YT"]), (T["Y2"], T["YT2"])]
                        for k in range(5):
                            Yn, YTn = nxt[k % 2]
                            b1 = newbank()
                            p.mm(b1.ap[:, 0:128], Y.ap, YT.ap, True, True, [Y.tk, YT.tk], [b1.tk])
                            evac("act", YTn.ap, [YTn.tk], b1.ap[:, 0:128], [b1.tk])
                            if k < 4:
                                b2 = newbank()
                                p.mm(b2.ap[:, 0:128], YT.ap, Y.ap, True, True, [Y.tk, YT.tk], [b2.tk])
                                evac("dve", Yn.ap, [Yn.tk], b2.ap[:, 0:128], [b2.tk])
                            b3 = newbank()
                            p.mm(b3.ap[:, 0:128], YTn.ap, T["Rm"].ap, True, True, [YTn.tk, T["Rm"].tk], [b3.tk])
                            p.call("dve", "tensor_tensor", [b3.tk, T["Rm"].tk], [T["Rm"].tk], out=T["Rm"].ap,
                                   in0=T["Rm"].ap, in1=b3.ap[:, 0:128], op=ALU.add)
                            Y, YT = Yn, YTn
                        if "gdnP3" in skip:
                            continue
                        bt = newbank()
                        p.tr(bt.ap[:, 0:128], ks, ident.ap, [kT[h].tk, ident.tk], [bt.tk])
                        evac("act", T["ktok"].ap, [T["ktok"].tk], bt.ap[:, 0:128], [bt.tk])
                        p.call("dve", "tensor_scalar", [kds.tk, pmask.tk], [kds.tk], out=kds.ap[:, 1:3],
                               in0=pmask.ap, scalar1=kds.ap[:, 0:1], scalar2=None, op0=ALU.mult)
                        p.call("dve", "tensor_scalar", [T["ktok"].tk, kds.tk], [T["kd"].tk], out=T["kd"].ap,
                               in0=T["ktok"].ap, scalar1=kds.ap[:, 1:2], scalar2=None, op0=ALU.mult)
                        p.call("dve", "tensor_scalar", [T["ktok"].tk, kds.tk], [T["kd1"].tk], out=T["kd1"].ap,
                               in0=T["ktok"].ap, scalar1=kds.ap[:, 2:3], scalar2=None, op0=ALU.mult)
                        bt2 = newbank()
                        p.tr(bt2.ap[:, 0:128], vs, ident.ap, [vT[h].tk, ident.tk], [bt2.tk])
                        evac("act", T["vtok"].ap, [T["vtok"].tk], bt2.ap[:, 0:128], [bt2.tk])
                        p.call("dve", "tensor_tensor", [qT[h].tk, T["egcb"].tk], [T["qg"].tk], out=T["qg"].ap, in0=qs,
                               in1=T["egcb"].ap, op=ALU.mult)
                        if "gdnP4" in skip:
                            continue
                        Sh = S_ap[:, h, :]
                        for ci in range(2):
                            r0 = ci * 64
                            rs = slice(r0, r0 + 64)
                            b1 = newbank()
                            p.mm(b1.ap[:, 0:128], ks, Sh, True, True, [kT[h].tk, S_tk[h]], [b1.tk])
                            p.call("dve", "scalar_tensor_tensor", [b1.tk, gsm.tk, T["vtok"].tk], [T["nr"].tk],
                                   out=T["nr"].ap[rs, :], in0=b1.ap[rs, 0:128], scalar=gs[rs, 20 + h:21 + h],
                                   in1=T["vtok"].ap[rs, :], op0=ALU.mult, op1=ALU.subtract)
                            b2 = newbank()
                            p.mm(b2.ap[:, 0:128], T["Rm"].ap, T["nr"].ap, True, True,
                                 [T["Rm"].tk, T["nr"].tk], [b2.tk])
                            p.call("dve", "tensor_scalar", [b2.tk, gsm.tk], [T["vnew"].tk], out=T["vnew"].ap[rs, :],
                                   in0=b2.ap[rs, 0:128], scalar1=gs[rs, 12 + h:13 + h], scalar2=None, op0=ALU.mult)
                            b3 = newbank()
                            p.mm(b3.ap[:, 0:128], T["qg"].ap, Sh, True, False, [T["qg"].tk, S_tk[h]], [b3.tk])
                            p.mm(b3.ap[:, 0:128], T["attnT"].ap, T["vnew"].ap, False, True,
                                 [T["attnT"].tk, T["vnew"].tk], [b3.tk])
                            evac("act", otok.ap[rs, h * 128:(h + 1) * 128], [otok.tk], b3.ap[rs, 0:128], [b3.tk])
                            b4 = newbank()
                            kdc = T["kd"] if ci == 0 else T["kd1"]
                            p.mm(b4.ap[:, 0:128], kdc.ap, T["vnew"].ap, True, True,
                                 [kdc.tk, T["vnew"].tk], [b4.tk])
                            p.call("dve", "scalar_tensor_tensor", [b4.tk, T["egcb"].tk, S_tk[h]], [S_tk[h]], out=Sh,
                                   in0=Sh, scalar=T["egcb"].ap[:, r0 + 63:r0 + 64], in1=b4.ap[:, 0:128],
                                   op0=ALU.mult, op1=ALU.add)
                    p.barrier()
                    for h in range(4 if "gdn2" not in skip else 0):
                        p.call("act", "activation", [otok.tk], [junk.tk, osq.tk], out=junk.ap[:, 0:128],
                               in_=otok.ap[:, h * 128:(h + 1) * 128], func=AF.Square, accum_out=osq.ap[:, h:h + 1])
                    if "gdn2" not in skip:
                        p.call("act", "activation", [osq.tk, lnst.tk], [osq.tk], out=osq.ap, in_=osq.ap, func=AF.Sqrt,
                               bias=lnst.ap[:, 9:10], scale=1.0 / 128.0)
                        p.call("dve", "reciprocal", [osq.tk], [osq.tk], out=osq.ap, in_=osq.ap)
                    for h in range(4 if "gdn2" not in skip else 0):
                        p.call("dve", "tensor_scalar", [otok.tk, osq.tk], [otok.tk],
                               out=otok.ap[:, h * 128:(h + 1) * 128], in0=otok.ap[:, h * 128:(h + 1) * 128],
                               scalar1=osq.ap[:, h:h + 1], scalar2=None, op0=ALU.mult)
                        bo = newbank()
                        p.tr(bo.ap[:, 0:128], otok.ap[:, h * 128:(h + 1) * 128], ident.ap, [otok.tk, ident.tk],
                             [bo.tk])
                        p.call("dve", "scalar_tensor_tensor", [bo.tk, sl_gnw.tk, zsT[h].tk], [ybr[0].tk],
                               out=R(ybr_ap[0][:, h, c0:c0 + 128]), in0=bo.ap[:, 0:128], scalar=sl_gnw.ap[:, 0:1],
                               in1=zsT[h].ap[:, c0:c0 + 128], op0=ALU.mult, op1=ALU.mult)

                    p.barrier()
                    ms = msm.ap
                    for h in range(4 if "mem" not in skip else 0):
                        bs = newbank()
                        p.mm(bs.ap[:, 0:256], mqT_ap[:, h, c0:c0 + 128], memKT_ap[:, h, :], True, True,
                             [mqT.tk, memKT.tk], [bs.tk])
                        p.call("dve", "reduce_max", [bs.tk], [msm.tk], out=ms[:, 0:1], in_=bs.ap[:, 0:256], axis=AX.X)
                        p.call("dve", "tensor_scalar", [msm.tk], [msm.tk], out=ms[:, 1:2], in0=ms[:, 0:1], scalar1=-1.0,
                               scalar2=None, op0=ALU.mult)
                        p.call("act", "activation", [bs.tk, msm.tk], [mP.tk, msm.tk], out=mP.ap, in_=bs.ap[:, 0:256],
                               func=AF.Exp, bias=ms[:, 1:2], scale=1.0, accum_out=ms[:, 2:3])
                        p.call("dve", "reciprocal", [msm.tk], [msm.tk], out=ms[:, 3:4], in_=ms[:, 2:3])
                        p.call("dve", "tensor_scalar", [mP.tk, msm.tk], [mP.tk], out=mP.ap, in0=mP.ap,
                               scalar1=ms[:, 3:4], scalar2=None, op0=ALU.mult)
                        bp = newbank()
                        for mt in range(2):
                            p.tr(bp.ap[:, mt * 128:(mt + 1) * 128], mP.ap[:, mt * 128:(mt + 1) * 128], ident.ap,
                                 [mP.tk, ident.tk], [bp.tk])
                        evac("act", mPT.ap, [mPT.tk], bp.ap[:, 0:256], [bp.tk])
                        by = newbank()
                        for mt in range(2):
                            p.mm(by.ap[:, 0:128], memV_ap[:, mt, h * 128:(h + 1) * 128],
                                 mPT.ap[:, mt * 128:(mt + 1) * 128], mt == 0, mt == 1, [memV.tk, mPT.tk], [by.tk])
                        p.call("dve", "tensor_copy", [by.tk], [ybr[2].tk], out=R(ybr_ap[2][:, h, c0:c0 + 128]),
                               in_=by.ap[:, 0:128])

                    p.barrier()
                    qb = tt * 2 + sub
                    Kq = (qb + 1) * 128
                    nseg = (Kq + 511) // 512
                    ztks = [banks[i].tk for i in range(nseg)]
                    zap = psum_t[:, 0:Kq]
                    ktk = KT_tk[0:tt + 1]
                    for hd in range(8 if "sb" not in skip else 0):
                        pr, hf = hd // 2, hd % 2
                        prs = slice(64 * hf, 64 * hf + 64)
                        for sg in range(nseg):
                            w = min(512, Kq - sg * 512)
                            p.mm(psum_t[:, sg * 512:sg * 512 + w], sqT_ap[prs, pr, c0:c0 + 128],
                                 KT_ap[prs, pr, sg * 512:sg * 512 + w], True, True, [sqT.tk] + ktk, [banks[sg].tk])
                        sp_ = sbsp.ap[:, 0:Kq]
                        C_ = sbC.ap[:, 0:Kq]
                        p.call("act", "activation", ztks, [sbsp.tk], out=sp_, in_=zap, func=AF.Exp)
                        p.call("act", "activation", [sbsp.tk], [sbsp.tk], out=sp_, in_=sp_, func=AF.Ln, bias=1.0,
                               scale=1.0)
                        p.call("pool", "tensor_tensor", [sbsp.tk, cm01.tk], [sbsp.tk], out=sbsp.ap[:, Kq - 128:Kq],
                               in0=sbsp.ap[:, Kq - 128:Kq], in1=cm01.ap, op=ALU.mult)
                        if "sbA" in skip:
                            continue
                        p.call("dve", "tensor_tensor_scan", [sbsp.tk, ones.tk], [sbC.tk], out=C_,
                               data0=ones.ap[:, 0:1].to_broadcast([128, Kq]), data1=sp_, initial=0.0, op0=ALU.mult,
                               op1=ALU.add)
                        p.call("dve", "tensor_scalar", [sbC.tk], [sbsm.tk], out=sbsm.ap[:, 0:1], in0=sbC.ap[:, Kq - 1:Kq],
                               scalar1=-1.0, scalar2=None, op0=ALU.mult)
                        if "sbB" in skip:
                            continue
                        p.call("pool", "tensor_tensor", [sbC.tk, sbsp.tk], [sbC.tk], out=C_, in0=C_, in1=sp_,
                               op=ALU.subtract)
                        p.call("dve", "tensor_tensor", [sbC.tk] + ztks, [sbC.tk], out=C_, in0=C_, in1=zap, op=ALU.add)
                        Wv = sbW_ap[:, 0:Kq]
                        p.call("act", "activation", [sbC.tk, sbsm.tk], [sbW.tk], out=Wv, in_=C_, func=AF.Exp,
                               bias=sbsm.ap[:, 0:1], scale=1.0)
                        p.call("pool", "tensor_tensor", [sbW.tk, cm01.tk], [sbW.tk], out=sbW_ap[:, Kq - 128:Kq],
                               in0=sbW_ap[:, Kq - 128:Kq], in1=cm01.ap, op=ALU.mult)
                        if "sbC" in skip:
                            continue
                        by = banks[7]
                        nkb = qb + 1
                        for g0 in range(0, nkb, 4):
                            gn = min(4, nkb - g0)
                            btr = newbank()
                            btr_b = btr.ap
                            for i in range(gn):
                                kb = g0 + i
                                p.tr(btr_b[:, i * 128:(i + 1) * 128], sbW_ap[:, kb * 128:(kb + 1) * 128], ident.ap,
                                     [sbW.tk, ident.tk], [btr.tk])
                            wt = sbWT[(g0 // 4) % 2]
                            wt_ap = wt.ap.bitcast(BF16)
                            if (g0 // 4) % 2 == 0:
                                p.call("act", "activation", [btr.tk], [wt.tk], out=wt_ap[:, 0:gn * 128],
                                       in_=btr_b[:, 0:gn * 128], func=AF.Copy)
                            else:
                                p.call("dve", "tensor_copy", [btr.tk], [wt.tk], out=wt_ap[:, 0:gn * 128],
                                       in_=btr_b[:, 0:gn * 128])
                            for i in range(gn if "sbD1" not in skip else 0):
                                kb = g0 + i
                                p.mm(by.ap[:, 0:128], V_ap[:, kb, pr * 128:(pr + 1) * 128],
                                     wt_ap[:, i * 128:(i + 1) * 128], kb == 0, kb == nkb - 1, [V_tk[kb], wt.tk],
                                     [by.tk])
                        if "sbD1" not in skip:
                            p.call("dve", "tensor_copy", [by.tk], [ybr[1].tk], out=R(ybr_ap[1][prs, pr, c0:c0 + 128]),
                                   in_=by.ap[prs, 0:128])

                if dbg and s == 0:
                    for brn in range(3):
                        dsl = p.slot("dbg%d_%d" % (tt, brn))
                        x1_stores.append(p.dma("sp", dbg_d[brn].rearrange("(k p) n -> p k n", p=128)[:, :, t0:t0 + TT],
                                               ybr_ap[brn], dsl, reads=[ybr[brn].tk]))
                def gate_handler(pi, c2, b):
                    gi = (pi - 16) * 2 + c2
                    dc, brn = gi // 3, gi % 3
                    p.call("act", "activation", [b.tk], [gsig.tk], out=gsig.ap, in_=b.ap[:, 0:TT], func=AF.Sigmoid)
                    wsl = wpring[wpr_i[0] % 3]
                    wpr_i[0] += 1
                    p.dma_in("sp", wsl.ap, wp_d[brn * 8 + dc], wsl)
                    w3 = wsl.ap.rearrange("p (k c) -> p k c", k=4)
                    b2 = newbank()
                    for kc in range(4):
                        p.mm(b2.ap[:, 0:TT], R(w3[:, kc, :]), R(ybr_ap[brn][:, kc, :]), kc == 0, kc == 3,
                             [wsl.tk, ybr[brn].tk], [b2.tk])
                    mo = R(mixedT_ap[:, dc, :])
                    mi = mixedT_ap[:, dc, :]
                    if brn == 0:
                        p.call("dve", "tensor_tensor", [gsig.tk, b2.tk], [mixedT.tk], out=mo, in0=gsig.ap,
                               in1=b2.ap[:, 0:TT], op=ALU.mult)
                    else:
                        p.call("dve", "tensor_tensor", [gsig.tk, b2.tk], [mixtmp.tk], out=mixtmp.ap, in0=gsig.ap,
                               in1=b2.ap[:, 0:TT], op=ALU.mult)
                        p.call("pool", "tensor_tensor", [mixtmp.tk, mixedT.tk], [mixedT.tk], out=mo, in0=mi,
                               in1=mixtmp.ap, op=ALU.add)

                for pi in range(16, 28 if "merge" not in skip else 16):
                    inproj_pair(pi, gate_handler)

                p.barrier()
                for i in range(4):
                    sl = ring_next()
                    p.dma_in("sp", sl.ap, wo_d[i], sl)
                    w3 = sl.ap.rearrange("p (k c) -> p k c", k=8)
                    for sub in range(2):
                        b = newbank()
                        for kc in range(8):
                            p.mm(b.ap[:, 0:256], R(mixedT_ap[:, kc, sub * 128:(sub + 1) * 128]), R(w3[:, kc, :]),
                                 kc == 0, kc == 7, [mixedT.tk, sl.tk], [b.tk])
                        p.call("dve", "scalar_tensor_tensor", [sl_x[sub].tk, b.tk], [rbuf[sub].tk],
                               out=rbuf[sub].ap[:, i * 256:(i + 1) * 256], in0=sl_x[sub].ap[:, i * 256:(i + 1) * 256],
                               scalar=ALPHA, in1=b.ap[:, 0:256], op0=ALU.mult, op1=ALU.add)
                for sub in range(2):
                    xo = rbuf[sub]
                    layer_norm(xo, sl_ln1g, sl_ln1b, xo.ap, [xo.tk])
                    x1_stores.append(p.dma("sp", x1_d[s, t0 + sub * 128:t0 + (sub + 1) * 128, :], xo.ap, xo,
                                           reads=[xo.tk]))

        p.barrier()
        aoff[0] = base_off
        bank_rr["lo"], bank_rr["n"], bank_rr["i"] = 0, 8, 0
        sl_ln2g = alloc_slot(1024, "ln2g")
        sl_ln2b = alloc_slot(1024, "ln2b")
        sl_wr = alloc_slot(256, "wr")
        sl_br = alloc_slot(32, "br")
        sl_bgu = alloc_slot(512, "bgu")
        sl_bd = alloc_slot(1024, "bd")
        p.dma_in("sp", sl_ln2g.ap, ln_d[2], sl_ln2g)
        p.dma_in("sp", sl_ln2b.ap, ln_d[3], sl_ln2b)
        p.dma_in("sp", sl_wr.ap, wr_d, sl_wr)
        p.dma_in("sp", sl_br.ap, br_d, sl_br)
        p.dma_in("sp", sl_bgu.ap, bgu_d, sl_bgu)
        p.call("pool", "memset", [], [sl_bd.tk], sl_bd.ap, 0.0)
        p.dma_in("sp", sl_bd.ap[0:32, :], bd_d, sl_bd)
        lnst = alloc(16, "lnst2")
        junk = alloc(1024, "junk2")
        p.call("pool", "memset", [], [lnst.tk], lnst.ap[:, 8:9], 1e-5)
        NSUB = G // 128
        x1T = alloc(8 * G, "x1T")
        x1T_ap = x1T.ap.rearrange("p (k n) -> p k n", k=8)
        yacc = [alloc_slot(1024, "yacc%d" % i) for i in range(NSUB)]
        gte = alloc(NSUB * 32, "gte")
        gte_ap = gte.ap.rearrange("p (a e) -> p a e", a=NSUB)
        actT = alloc(8 * G, "actT")
        actT_ap = actT.ap.rearrange("p (j n) -> p j n", j=8)
        wgu = [alloc_slot(2048, "wgu%d" % i) for i in range(3)]
        wgu_i = [0]
        sl_wd = alloc_slot(8192, "wd")
        wd_ap = sl_wd.ap.rearrange("p (f d) -> p f d", f=8)
        x1in = [alloc_slot(1024, "x1in%d" % i) for i in range(2)]
        x1T32 = alloc(1024, "x1T32")
        x1T32_ap = x1T32.ap.rearrange("p (k n) -> p k n", k=8)
        rt = alloc(128, "rt")
        gTs = alloc(128, "gTs")
        gpad = alloc(128, "gpad")
        p.call("pool", "memset", [], [gpad.tk], gpad.ap, 0.0)
        tglu = [alloc(512, "tglu%d" % i) for i in range(2)]
        tsig = [alloc(512, "tsig%d" % i) for i in range(2)]
        tlin = [alloc(512, "tlin%d" % i) for i in range(2)]
        print("phase B arena use", aoff[0])
        final = []
        outb_i = [0]
        tmp_i = [0]

        for grp in range((ngrp or nseq * (S // G)) if phase_b else 0):
            s, gh = grp // (S // G), grp % (S // G)
            tok0 = gh * G
            for sub in range(NSUB):
                p.barrier()
                xi = x1in[sub % 2]
                p.dma_in("sp", xi.ap, x1_d[s, tok0 + sub * 128:tok0 + (sub + 1) * 128, :], xi)
                ya = yacc[sub]
                p.call("act", "activation", [xi.tk], [ya.tk], out=ya.ap, in_=xi.ap, func=AF.Copy, scale=ALPHA)
                for half in range(2):
                    b = newbank()
                    for i in range(4):
                        kc = half * 4 + i
                        p.tr(b.ap[:, i * 128:(i + 1) * 128], xi.ap[:, kc * 128:(kc + 1) * 128], ident.ap,
                             [xi.tk, ident.tk], [b.tk])
                    for i in range(4):
                        kc = half * 4 + i
                        p.call("act", "activation", [b.tk], [x1T32.tk], out=x1T32_ap[:, kc, :],
                               in_=b.ap[:, i * 128:(i + 1) * 128], func=AF.Copy)
                        p.call("dve", "tensor_copy", [x1T32.tk], [x1T.tk],
                               out=R(x1T_ap[:, kc, sub * 128:(sub + 1) * 128]), in_=x1T32_ap[:, kc, :])
                p.barrier()
                r_ = rt.ap
                bl = newbank()
                wr3 = sl_wr.ap.rearrange("p (k e) -> p k e", k=8)
                for kc in range(8):
                    p.mm(bl.ap[:, 0:32], x1T32_ap[:, kc, :], wr3[:, kc, :], kc == 0, kc == 7, [x1T32.tk, sl_wr.tk],
                         [bl.tk])
                p.call("dve", "tensor_tensor", [bl.tk, sl_br.tk], [rt.tk], out=r_[:, 0:32], in0=bl.ap[:, 0:32],
                       in1=sl_br.ap, op=ALU.add)
                p.call("dve", "tensor_copy", [rt.tk], [rt.tk], out=r_[:, 40:72], in_=r_[:, 0:32])
                for i4 in range(4):
                    p.call("dve", "reduce_max", [rt.tk], [rt.tk], out=r_[:, 32 + i4:33 + i4], in_=r_[:, 40:72], axis=AX.X)
                    if i4 < 3:
                        p.call("dve", "tensor_scalar", [rt.tk], [rt.tk], out=r_[:, 80:112], in0=r_[:, 40:72],
                               scalar1=r_[:, 32 + i4:33 + i4], scalar2=None, op0=ALU.is_equal)
                        p.call("dve", "scalar_tensor_tensor", [rt.tk], [rt.tk], out=r_[:, 40:72], in0=r_[:, 80:112],
                               scalar=-1e30, in1=r_[:, 40:72], op0=ALU.mult, op1=ALU.add)
                p.call("dve", "tensor_scalar", [rt.tk], [rt.tk], out=r_[:, 40:72], in0=r_[:, 0:32], scalar1=r_[:, 35:36],
                       scalar2=None, op0=ALU.is_ge)
                p.call("dve", "tensor_scalar", [rt.tk], [rt.tk], out=r_[:, 72:73], in0=r_[:, 32:33], scalar1=-1.0,
                       scalar2=None, op0=ALU.mult)
                p.call("act", "activation", [rt.tk], [rt.tk], out=r_[:, 80:112], in_=r_[:, 0:32], func=AF.Exp,
                       bias=r_[:, 72:73], scale=1.0)
                p.call("dve", "tensor_tensor", [rt.tk], [rt.tk], out=r_[:, 80:112], in0=r_[:, 80:112], in1=r_[:, 40:72],
                       op=ALU.mult)
                p.call("dve", "reduce_sum", [rt.tk], [rt.tk], out=r_[:, 73:74], in_=r_[:, 80:112], axis=AX.X)
                p.call("dve", "reciprocal", [rt.tk], [rt.tk], out=r_[:, 74:75], in_=r_[:, 73:74])
                p.call("dve", "tensor_scalar", [rt.tk], [gte.tk], out=gte_ap[:, sub, :], in0=r_[:, 80:112],
                       scalar1=r_[:, 74:75], scalar2=None, op0=ALU.mult)
                p.barrier()
                bg = newbank()
                p.call("dve", "tensor_copy", [gte.tk], [gpad.tk], out=gpad.ap[:, 0:32], in_=gte_ap[:, sub, :])
                p.tr(bg.ap[:, 0:128], gpad.ap, ident.ap, [gpad.tk, ident.tk], [bg.tk])
                evac("act", gTs.ap, [gTs.tk], bg.ap[:, 0:128], [bg.tk])
                for half in range(2):
                    bb = newbank()
                    p.mm(bb.ap[:, 0:512], gTs.ap, sl_bd.ap[:, half * 512:(half + 1) * 512], True, True,
                         [gTs.tk, sl_bd.tk], [bb.tk])
                    p.call("dve", "tensor_tensor", [bb.tk, ya.tk], [ya.tk], out=ya.ap[:, half * 512:(half + 1) * 512],
                           in0=ya.ap[:, half * 512:(half + 1) * 512], in1=bb.ap[:, 0:512], op=ALU.add)

            for e in range(nexp):
                p.barrier()
                p.dma_in("sp", sl_wd.ap, wd_d[e], sl_wd)
                for j in range(8):
                    wsl = wgu[wgu_i[0] % 3]
                    wgu_i[0] += 1
                    p.dma_in("sp", wsl.ap, wgu_d[e * 8 + j], wsl)
                    w3 = wsl.ap.rearrange("p (k c) -> p k c", k=8)
                    for t5 in range(G // 512):
                        cs = slice(t5 * 512, (t5 + 1) * 512)
                        bgt = newbank()
                        for kc in range(8):
                            p.mm(bgt.ap, R(w3[:, kc, 0:128]), R(x1T_ap[:, kc, cs]), kc == 0, kc == 7,
                                 [wsl.tk, x1T.tk], [bgt.tk])
                        blt = newbank()
                        for kc in range(8):
                            p.mm(blt.ap, R(w3[:, kc, 128:256]), R(x1T_ap[:, kc, cs]), kc == 0, kc == 7,
                                 [wsl.tk, x1T.tk], [blt.tk])
                        ti = tmp_i[0] % 2
                        tmp_i[0] += 1
                        bgc = sl_bgu.ap[:, e * 16 + j:e * 16 + j + 1]
                        blc = sl_bgu.ap[:, e * 16 + 8 + j:e * 16 + 8 + j + 1]
                        p.call("dve", "tensor_scalar", [bgt.tk, sl_bgu.tk], [tglu[ti].tk], out=tglu[ti].ap, in0=bgt.ap,
                               scalar1=bgc, scalar2=cst.ap[:, 1:2], op0=ALU.add, op1=ALU.min)
                        p.call("act", "activation", [tglu[ti].tk], [tsig[ti].tk], out=tsig[ti].ap, in_=tglu[ti].ap,
                               func=AF.Sigmoid, scale=1.702)
                        p.call("dve", "tensor_scalar", [blt.tk, sl_bgu.tk], [tlin[ti].tk], out=tlin[ti].ap, in0=blt.ap,
                               scalar1=blc, scalar2=cst.ap[:, 1:2], op0=ALU.add, op1=ALU.min)
                        p.call("pool", "tensor_scalar", [tlin[ti].tk], [tlin[ti].tk], out=tlin[ti].ap, in0=tlin[ti].ap,
                               scalar1=-7.0, scalar2=1.0, op0=ALU.max, op1=ALU.add)
                        p.call("pool", "tensor_tensor", [tglu[ti].tk, tsig[ti].tk], [tglu[ti].tk], out=tglu[ti].ap,
                               in0=tglu[ti].ap, in1=tsig[ti].ap, op=ALU.mult)
                        p.call("pool", "tensor_tensor", [tglu[ti].tk, tlin[ti].tk], [actT.tk], out=R(actT_ap[:, j, cs]),
                               in0=tglu[ti].ap, in1=tlin[ti].ap, op=ALU.mult)
                p.barrier()
                for sub in range(NSUB):
                    for half in range(2):
                        bo = newbank()
                        for j in range(8):
                            p.mm(bo.ap, R(actT_ap[:, j, sub * 128:(sub + 1) * 128]),
                                 R(wd_ap[:, j, half * 512:(half + 1) * 512]), j == 0, j == 7, [actT.tk, sl_wd.tk],
                                 [bo.tk])
                        ya = yacc[sub]
                        p.call("dve", "scalar_tensor_tensor", [bo.tk, gte.tk, ya.tk], [ya.tk],
                               out=ya.ap[:, half * 512:(half + 1) * 512], in0=bo.ap, scalar=gte_ap[:, sub, e:e + 1],
                               in1=ya.ap[:, half * 512:(half + 1) * 512], op0=ALU.mult, op1=ALU.add)

            for sub in range(NSUB):
                p.barrier()
                ob = yacc[sub]
                layer_norm(ob, sl_ln2g, sl_ln2b, ob.ap, [ob.tk])
                final.append(p.dma("sp", out_d[s, tok0 + sub * 128:tok0 + (sub + 1) * 128, :], ob.ap, ob,
                                   reads=[ob.tk]))

        p.emit(final_waits=(final[-NSUB:] if phase_b else x1_stores))
        print("ops", p.stats)
    return nc


_CACHE = {}


def _prep_shared(w_in, w_conv, a_log, dt_bias, gdn_norm_w, w_mem_kv, w_p_gdn, w_p_sb, w_p_mem, w_o, ln1_g, ln1_b,
                 w_router, b_router, w_gate_up, b_gate_up, w_down, b_down, ln2_g, ln2_b):
    f = np.float32
    w_in = np.asarray(w_in[0], f)

    def kp(wcols):
        return wcols.reshape(8, 128, -1).transpose(1, 0, 2)

    chunks = []
    for base in (0, 512, 1024, 1536):
        for h in range(4):
            chunks.append(np.arange(base + h * 128, base + (h + 1) * 128))
    for base in (2056, 2568, 3080, 3592):
        for h in range(4):
            chunks.append(np.arange(base + h * 128, base + (h + 1) * 128))
    for dc in range(8):
        for br in range(3):
            chunks.append(np.arange(4104 + br * 1024 + dc * 128, 4104 + br * 1024 + (dc + 1) * 128))
    assert len(chunks) == 56
    wfm = np.empty((28, 128, 8, 256), f)
    for pi in range(28):
        cols = np.concatenate([chunks[2 * pi], chunks[2 * pi + 1]])
        wfm[pi] = kp(w_in[:, cols])
    d = {}
    d["wfm"] = wfm.reshape(28, 128, 2048)
    d["wab"] = np.ascontiguousarray(kp(w_in[:, 2048:2056])).reshape(128, 64)
    wc = np.asarray(w_conv[0], f)
    d["wconv"] = np.ascontiguousarray(wc.reshape(4, 12, 128).transpose(2, 1, 0)).reshape(128, 48)
    d["alog"] = np.ascontiguousarray(np.broadcast_to(np.asarray(a_log[0], f)[None, :], (128, 4)))
    d["dtb"] = np.ascontiguousarray(np.broadcast_to(np.asarray(dt_bias[0], f)[None, :], (128, 4)))
    d["gnw"] = np.ascontiguousarray(np.asarray(gdn_norm_w[0], f).reshape(128, 1))
    wm = np.asarray(w_mem_kv[0], f)
    d["wmkv"] = np.ascontiguousarray(np.stack([kp(wm[:, i * 256:(i + 1) * 256]) for i in range(4)])).reshape(4, 128, 2048)
    wps = [np.asarray(w[0], f) for w in (w_p_gdn, w_p_sb, w_p_mem)]
    wp = np.empty((24, 128, 4, 128), f)
    for br in range(3):
        for dc in range(8):
            wp[br * 8 + dc] = wps[br][:, dc * 128:(dc + 1) * 128].reshape(4, 128, 128).transpose(1, 0, 2)
    d["wp"] = wp.reshape(24, 128, 512)
    wo = np.asarray(w_o[0], f)
    d["wo"] = np.ascontiguousarray(np.stack([kp(wo[:, i * 256:(i + 1) * 256]) for i in range(4)])).reshape(4, 128, 2048)
    d["ln"] = np.ascontiguousarray(np.stack([np.broadcast_to(np.asarray(v[0], f)[None, :], (128, 1024))
                                             for v in (ln1_g, ln1_b, ln2_g, ln2_b)]))
    d["wr"] = np.ascontiguousarray(kp(np.asarray(w_router[0], f))).reshape(128, 256)
    d["br"] = np.ascontiguousarray(np.broadcast_to(np.asarray(b_router[0], f)[None, :], (128, 32)))
    wg = np.asarray(w_gate_up[0], f)
    wgu = np.empty((32, 8, 128, 8, 256), f)
    w5 = wg.reshape(32, 8, 128, 2, 8, 128)
    wgu[:] = w5.transpose(0, 4, 2, 1, 3, 5).reshape(32, 8, 128, 8, 256)
    d["wgu"] = wgu.reshape(256, 128, 2048)
    bg = np.asarray(b_gate_up[0], f)
    d["bgu"] = np.ascontiguousarray(bg.reshape(32, 16, 128).transpose(2, 0, 1)).reshape(128, 512)
    wd = np.asarray(w_down[0], f)
    d["wd"] = np.ascontiguousarray(wd.reshape(32, 8, 128, 1024).transpose(0, 2, 1, 3)).reshape(32, 128, 8192)
    d["bd"] = np.ascontiguousarray(np.asarray(b_down[0], f))
    return d


def kernel(x, mem, **w):
    x = np.asarray(x, np.float32)
    mem = np.asarray(mem, np.float32)
    shared = _prep_shared(**w)
    if "nc" not in _CACHE:
        _CACHE["nc"] = build_program()
    nc = _CACHE["nc"]
    in_maps = []
    for c in range(8):
        xb = x[2 * c:2 * c + 2]
        mb = mem[2 * c:2 * c + 2]
        m = dict(shared)
        m["x"] = np.ascontiguousarray(xb)
        m["xT"] = np.ascontiguousarray(xb.transpose(0, 2, 1))
        m["memT"] = np.ascontiguousarray(mb.transpose(0, 2, 1))
        in_maps.append(m)
    res = run_bass_kernel_spmd(nc, in_maps, core_ids=list(range(8)))
    out = np.concatenate([r["out"] for r in res.results], axis=0)
    return out.astype(np.float32)
```
